# Optimizing a Trainium2 kernel written in Bass

```python
import jax, jax.numpy as jnp
from jax import lax
import numpy as np

D_MODEL = 1024
BATCH = 2
SEQ = 8192
DEPTH = 4

D_A = D_MODEL
CONV_A = 31
D_B = D_MODEL
CONV_B = 3
SPLITS = (D_A, 2 * D_A, 2 * D_A + D_B, 2 * D_A + 2 * D_B, 2 * D_A + 3 * D_B,
          2 * D_A + 3 * D_B + D_MODEL)
N_IN = 2 * D_A + 3 * D_B + 2 * D_MODEL
N_EXPERTS = 32
TOP_K = 4
D_FF = D_MODEL
SWIGLU_LIMIT = 7.0
SWIGLU_ALPHA = 1.702
EXPERT_BLOCK = 256
DEEPNORM_ALPHA = (2.0 * DEPTH) ** 0.25
DEEPNORM_BETA = (8.0 * DEPTH) ** -0.25
LN_EPS = 1e-5

kernel_name = "hybrid_conformer_shortconv_moe_deepnorm_adaln"


def layer_norm(x, g=None, b=None):
    xf = x.astype(jnp.float32)
    mu = jnp.mean(xf, axis=-1, keepdims=True)
    xc = xf - mu
    var = jnp.mean(jnp.square(xc), axis=-1, keepdims=True)
    y = xc * lax.rsqrt(var + LN_EPS)
    if g is not None:
        y = y * g.astype(jnp.float32) + b.astype(jnp.float32)
    return y.astype(x.dtype)


def causal_dwconv(x, w):
    k, ch = w.shape
    return lax.conv_general_dilated(
        x, w[:, None, :].astype(x.dtype), window_strides=(1,), padding=[(k - 1, 0)],
        dimension_numbers=("NWC", "WIO", "NWC"), feature_group_count=ch)


def token_mixer(u, w_in, b_in, conv_a_w, conv_a_b, ln_a_g, ln_a_b, conv_b_w, w_pa, b_pa, w_pb, w_o):
    z = u @ w_in + b_in
    a_val, a_gate, gb, gc, hb, z_ga, z_gb = jnp.split(z, SPLITS, axis=-1)
    ya = a_val * jax.nn.sigmoid(a_gate)
    ya = causal_dwconv(ya, conv_a_w) + conv_a_b
    ya = jax.nn.silu(layer_norm(ya, ln_a_g, ln_a_b))
    ya = ya @ w_pa + b_pa
    yb = (gb * causal_dwconv(gc * hb, conv_b_w)) @ w_pb
    m = jax.nn.sigmoid(z_ga) * ya + jax.nn.sigmoid(z_gb) * yb
    return m @ w_o


def clamped_swiglu(h):
    h_glu, h_lin = jnp.split(h, 2, axis=-1)
    h_glu = jnp.minimum(h_glu, SWIGLU_LIMIT)
    h_lin = jnp.clip(h_lin, -SWIGLU_LIMIT, SWIGLU_LIMIT)
    return h_glu * jax.nn.sigmoid(SWIGLU_ALPHA * h_glu) * (h_lin + 1.0)


def moe(u, router_w, router_b, w1, b1, w2, b2):
    bsz, seq, d = u.shape
    n_tok = bsz * seq
    xf = u.reshape(n_tok, d)
    logits = (xf @ router_w + router_b).astype(jnp.float32)
    top_v, top_i = lax.top_k(logits, TOP_K)
    gate = jax.nn.softmax(top_v, axis=-1).astype(u.dtype)
    n_asg = n_tok * TOP_K
    asg_e = top_i.reshape(n_asg).astype(jnp.int32)
    asg_tok = jnp.arange(n_asg, dtype=jnp.int32) // TOP_K
    order = jnp.argsort(asg_e, stable=True)
    sorted_e = asg_e[order]
    counts = jnp.bincount(asg_e, length=N_EXPERTS).astype(jnp.int32)
    padded = (counts + EXPERT_BLOCK - 1) // EXPERT_BLOCK * EXPERT_BLOCK
    pend = jnp.cumsum(padded)
    pstart = pend - padded
    ustart = jnp.cumsum(counts) - counts
    dest = pstart[sorted_e] + jnp.arange(n_asg, dtype=jnp.int32) - ustart[sorted_e]
    n_blocks = (n_asg + N_EXPERTS * (EXPERT_BLOCK - 1) + EXPERT_BLOCK - 1) // EXPERT_BLOCK
    n_rows = n_blocks * EXPERT_BLOCK
    row_tok = jnp.zeros((n_rows,), jnp.int32).at[dest].set(asg_tok[order])
    row_w = jnp.zeros((n_rows,), u.dtype).at[dest].set(gate.reshape(n_asg)[order])
    block_e = jnp.minimum(
        jnp.searchsorted(pend, jnp.arange(n_blocks, dtype=jnp.int32) * EXPERT_BLOCK, side="right"),
        N_EXPERTS - 1)
    xs = xf[row_tok].reshape(n_blocks, EXPERT_BLOCK, d)

    def expert_block(args):
        xb, e = args
        h = clamped_swiglu(xb @ w1[e] + b1[e])
        return h @ w2[e] + b2[e]

    ys = lax.map(expert_block, (xs, block_e)).reshape(n_rows, d)
    y = jax.ops.segment_sum(ys * row_w[:, None], row_tok, num_segments=n_tok)
    return y.reshape(bsz, seq, d)


def setup_inputs(seed: int = 0) -> dict:
    key = jax.random.key(seed)
    ks = jax.random.split(key, 32)
    L, D, E, F = DEPTH, D_MODEL, N_EXPERTS, D_FF
    nrm = lambda k, shape, s: jax.random.normal(k, shape, jnp.float32) * s
    gain = lambda k, shape: 1.0 + 0.01 * jax.random.normal(k, shape, jnp.float32)
    return {
        "x": nrm(ks[0], (BATCH, SEQ, D), 1.0),
        "c": nrm(ks[1], (BATCH, D), 1.0),
        "ada_w": nrm(ks[2], (L, D, 6 * D), D ** -0.5),
        "ada_b": nrm(ks[3], (L, 6 * D), 0.01),
        "w_in": nrm(ks[4], (L, D, N_IN), D ** -0.5),
        "b_in": nrm(ks[5], (L, N_IN), 0.01),
        "conv_a_w": nrm(ks[6], (L, CONV_A, D_A), CONV_A ** -0.5),
        "conv_a_b": nrm(ks[7], (L, D_A), 0.01),
        "ln_a_g": gain(ks[8], (L, D_A)),
        "ln_a_b": nrm(ks[9], (L, D_A), 0.01),
        "conv_b_w": nrm(ks[10], (L, CONV_B, D_B), CONV_B ** -0.5),
        "w_pa": nrm(ks[11], (L, D_A, D), D_A ** -0.5 * DEEPNORM_BETA),
        "b_pa": nrm(ks[12], (L, D), 0.01),
        "w_pb": nrm(ks[13], (L, D_B, D), D_B ** -0.5 * DEEPNORM_BETA),
        "w_o": nrm(ks[14], (L, D, D), D ** -0.5 * DEEPNORM_BETA),
        "ln1_g": gain(ks[15], (L, D)),
        "ln1_b": nrm(ks[16], (L, D), 0.01),
        "router_w": nrm(ks[17], (L, D, E), D ** -0.5),
        "router_b": nrm(ks[18], (L, E), 0.01),
        "w1": nrm(ks[19], (L, E, D, 2 * F), D ** -0.5),
        "b1": nrm(ks[20], (L, E, 2 * F), 0.01),
        "w2": nrm(ks[21], (L, E, F, D), F ** -0.5 * DEEPNORM_BETA),
        "b2": nrm(ks[22], (L, E, D), 0.01),
        "ln2_g": gain(ks[23], (L, D)),
        "ln2_b": nrm(ks[24], (L, D), 0.01),
    }


def reference(x, c, ada_w, ada_b, w_in, b_in, conv_a_w, conv_a_b, ln_a_g, ln_a_b, conv_b_w,
              w_pa, b_pa, w_pb, w_o, ln1_g, ln1_b, router_w, router_b, w1, b1, w2, b2,
              ln2_g, ln2_b):
    cond = jax.nn.silu(c)
    for l in range(DEPTH):
        mod = cond @ ada_w[l] + ada_b[l]
        sh1, sc1, g1, sh2, sc2, g2 = [m[:, None, :] for m in jnp.split(mod, 6, axis=-1)]
        u = layer_norm(x) * (1.0 + sc1) + sh1
        y = token_mixer(u, w_in[l], b_in[l], conv_a_w[l], conv_a_b[l], ln_a_g[l], ln_a_b[l],
                        conv_b_w[l], w_pa[l], b_pa[l], w_pb[l], w_o[l])
        x = layer_norm(DEEPNORM_ALPHA * x + g1 * y, ln1_g[l], ln1_b[l])
        u = layer_norm(x) * (1.0 + sc2) + sh2
        y = moe(u, router_w[l], router_b[l], w1[l], b1[l], w2[l], b2[l])
        x = layer_norm(DEEPNORM_ALPHA * x + g2 * y, ln2_g[l], ln2_b[l])
    return x
```

```python
from contextlib import ExitStack
import numpy as np
import concourse.bass as bass
import concourse.mybir as mybir
from concourse.bass_utils import run_bass_kernel_spmd

F32 = mybir.dt.float32
BF16 = mybir.dt.bfloat16
AF = mybir.ActivationFunctionType
ALU = mybir.AluOpType

D = 1024
KC = 8
NE = 32
DEPTH = 4
ALPHA = (2.0 * DEPTH) ** 0.25
EPS = 1e-5
HALO = 128
N_IN = 7168

C_BIN = 0
C_CAW = 56
C_CAB = 304
C_LAG = 312
C_LAB = 320
C_CBW = 328
C_BPA = 352
C_L1G = 360
C_L1B = 368
C_L2G = 376
C_L2B = 384
C_ADB = 392
C_B1 = 440
NCOL = 952

ENGS = ("pe", "act", "dve", "pool", "sp")


class Buf:
    def __init__(self, name, handle, excl=False):
        self.excl = excl
        self.name = name
        self.h = handle
        self.last_w = None
        self.readers = []

    def __getitem__(self, k):
        return self.h[k]


class FW:
    def __init__(self, nc):
        self.nc = nc
        self.es = ExitStack()
        self.E = {"pe": nc.tensor, "act": nc.scalar, "dve": nc.vector, "pool": nc.gpsimd, "sp": nc.sync}
        self.sem = {e: self.es.enter_context(nc.semaphore("s_" + e)) for e in ENGS}
        self.tick = {e: 0 for e in ENGS}
        self.seen = {e: {} for e in ENGS}
        self.dsems = {}
        self.dcnt = {}

    def sbuf(self, name, shape, dt):
        h = self.es.enter_context(self.nc.sbuf_tensor(name, list(shape), dt))
        return Buf(name, h)

    def psum(self, name, shape, dt):
        h = self.es.enter_context(self.nc.psum_tensor(name, list(shape), dt))
        return Buf(name, h, excl=True)

    def dsem(self, name):
        self.dsems[name] = self.es.enter_context(self.nc.semaphore("d_" + name))
        self.dcnt[name] = 0
        return name

    def _wait(self, e, dep):
        kind, key, tick = dep
        if kind == "e" and key == e and e == "pe":
            return
        sem = self.sem[key] if kind == "e" else self.dsems[key]
        sk = (kind, key)
        if self.seen[e].get(sk, 0) >= tick:
            return
        self.E[e].wait_ge(sem, tick)
        self.seen[e][sk] = tick

    def deps(self, e, reads, writes):
        for b in reads:
            if b.last_w is not None:
                self._wait(e, b.last_w)
            if b.excl:
                for r in b.readers:
                    if not (r[0] == "e" and r[1] == e):
                        self._wait(e, r)
        for b in writes:
            if b.last_w is not None:
                self._wait(e, b.last_w)
            for r in b.readers:
                self._wait(e, r)

    def done(self, e, ins, reads, writes, mark=True):
        if mark:
            self.tick[e] += 1
            ins.then_inc(self.sem[e], 1)
            t = self.tick[e]
        else:
            t = self.tick[e] + 1
        d = ("e", e, t)
        for b in reads:
            b.readers = [r for r in b.readers if not (r[0] == "e" and r[1] == e)] + [d]
        for b in writes:
            b.last_w = d
            b.readers = []
        return ins

    def op(self, e, fn, reads, writes, mark=True):
        self.deps(e, reads, writes)
        ins = fn()
        return self.done(e, ins, reads, writes, mark)

    def dma(self, q, out_ap, in_ap, reads=(), writes=(), dname=None):
        self.deps(q, reads, writes)
        ins = self.E[q].dma_start(out=out_ap, in_=in_ap)
        self.dcnt[dname] += 16
        ins.then_inc(self.dsems[dname], 16)
        d = ("d", dname, self.dcnt[dname])
        for b in reads:
            b.readers = b.readers + [d]
        for b in writes:
            b.last_w = d
            b.readers = []
        return d

    def close(self):
        self.es.close()


class Rot:
    def __init__(self, bufs):
        self.bufs = bufs
        self.i = 0

    def next(self):
        b = self.bufs[self.i % len(self.bufs)]
        self.i += 1
        return b


def blocks_of(n):
    nb = (n + 511) // 512
    base = -(-n // nb)
    base = -(-base // 64) * 64
    out, o = [], 0
    while o < n:
        s = min(base, n - o)
        out.append((o, s))
        o += s
    return out


def build_program(L, T, GT, NEXP=NE, DBG=""):
    assert T % GT == 0 and GT % 64 == 0
    NG = T // GT
    TOWN = T - HALO
    nc = bass.Bass("TRN2", target_bir_lowering=False)
    dt_ = nc.dram_tensor
    xT_d = dt_("xT", [128, KC, T], F32, kind="ExternalInput").ap()
    cT_d = dt_("cT", [128, KC], F32, kind="ExternalInput").ap()
    mask_d = dt_("mask", [128, 128], F32, kind="ExternalInput").ap()
    ident_d = dt_("ident", [128, 128], F32, kind="ExternalInput").ap()
    cols_d = dt_("cols", [L, 128, NCOL], F32, kind="ExternalInput").ap()
    adaw_d = dt_("ada_w", [L, D, 6 * D], F32, kind="ExternalInput").ap()
    win_d = dt_("w_in", [L, D, N_IN], F32, kind="ExternalInput").ap()
    wpa_d = dt_("w_pa", [L, D, D], F32, kind="ExternalInput").ap()
    wpb_d = dt_("w_pb", [L, D, D], F32, kind="ExternalInput").ap()
    wo_d = dt_("w_o", [L, D, D], F32, kind="ExternalInput").ap()
    rw_d = dt_("router_w", [L, D, NE], F32, kind="ExternalInput").ap()
    rb_d = dt_("router_b", [L, NE], F32, kind="ExternalInput").ap()
    w1_d = dt_("w1", [L, NE, D, 2 * D], F32, kind="ExternalInput").ap()
    w2_d = dt_("w2", [L, NE, D, D], F32, kind="ExternalInput").ap()
    b2_d = dt_("b2", [L, NE, D], F32, kind="ExternalInput").ap()
    yT_d = dt_("yT", [128, KC, TOWN], F32, kind="ExternalOutput").ap()

    fw = FW(nc)
    V, A, G, PE = nc.vector, nc.scalar, nc.gpsimd, nc.tensor

    xs = fw.sbuf("xs", [128, KC, T], F32)
    uT = fw.sbuf("uT", [128, KC, GT], BF16)
    yab = fw.sbuf("yab", [128, KC, GT], BF16)
    yabB = [Buf(f"yab_b{i}", yab.h) for i in range(len(blocks_of(GT)))]
    NS = 3
    slots = [fw.sbuf(f"slot{i}", [128, KC, 1024], BF16) for i in range(NS)]
    arena = fw.sbuf("arena", [128, 4096], F32)
    assert 3 * GT + 30 <= 4096 and len(blocks_of(GT)) <= 3
    ypad_v = arena[:, 0:30 + GT]
    acc_v = arena[:, 30 + GT:30 + 2 * GT]
    gbt_v = arena[:, 30 + 2 * GT:30 + 3 * GT]
    YPW = (30 + GT) // 2
    ypb_v = arena[:, 0:YPW].bitcast(BF16)
    dg_v = arena[:, YPW:YPW + 31 * 64].bitcast(BF16).rearrange("p (k m) -> p k m", k=31)
    assert YPW + 31 * 64 <= 4096
    mb_v = arena[:, 0:2048].bitcast(BF16).rearrange("p (c n) -> p c n", c=KC)
    sbf_v = arena[:, 2048:4096].bitcast(BF16).rearrange("p (c n) -> p c n", c=KC)
    gateT_v = arena[0:NE, 0:GT]
    b2t_v = arena[0:NE, GT:GT + D]
    GE0 = GT + D
    ge_v = [arena[:, GE0 + i * 512:GE0 + (i + 1) * 512] for i in range(3)]
    assert GE0 + 3 * 512 <= 4096
    cols = fw.sbuf("cols_sb", [128, NCOL], F32)
    modT = fw.sbuf("modT", [128, L * 48], F32)
    op1 = fw.sbuf("op1", [128, L * 16], F32)
    b1s = fw.sbuf("b1s", [128, NE * 16], F32)
    ident = fw.sbuf("ident_sb", [128, 128], F32)
    maskt = fw.sbuf("maskt", [128, 128], F32)
    ones_f = fw.sbuf("ones_f", [128, 128], F32)
    ones_b = fw.sbuf("ones_b", [128, 128], BF16)
    epsb = fw.sbuf("epsb", [128, 1], F32)
    c119 = fw.sbuf("c119", [128, 1], F32)
    c7 = fw.sbuf("c7", [128, 1], F32)
    condf = fw.sbuf("condf", [128, KC], F32)
    condb = fw.sbuf("condb", [128, KC], BF16)
    histA = fw.sbuf("histA", [128, KC, 30], BF16)
    histB = fw.sbuf("histB", [128, KC, 2], F32)
    rwb = fw.sbuf("rwb", [128, KC, NE], BF16)
    rbb = fw.sbuf("rbb", [1, NE], BF16)
    tmpA = Rot([fw.sbuf(f"tmpA{i}", [128, 512], F32) for i in range(2)])
    tmpD = Rot([fw.sbuf(f"tmpD{i}", [128, 512], F32) for i in range(2)])
    tmpP = Rot([fw.sbuf(f"tmpP{i}", [128, 512], F32) for i in range(2)])
    tmpL = Rot([fw.sbuf(f"tmpL{i}", [128, 512], F32) for i in range(2)])
    meanT = fw.sbuf("meanT", [128, 512], F32)
    rstdT = fw.sbuf("rstdT", [128, 512], F32)
    msqT = fw.sbuf("msqT", [128, 512], F32)
    meanT2 = fw.sbuf("meanT2", [128, 512], F32)
    rstdT2 = fw.sbuf("rstdT2", [128, 512], F32)
    msqT2 = fw.sbuf("msqT2", [128, 512], F32)
    ident_b = fw.sbuf("ident_b", [128, 128], BF16)
    small = Rot([fw.sbuf(f"small{i}", [128, 96], F32) for i in range(2)])
    pb = [fw.psum(f"pb{i}", [128, 512], F32) for i in range(8)]
    prot = Rot(pb[0:6])
    pS1, pS2 = pb[6], pb[7]

    for n in ("in", "cols", "w0", "w1", "w2", "ident", "mask", "cond", "rwb", "rbb", "b2t", "out"):
        fw.dsem(n)

    jobs = []
    state = {"issued": 0}

    def issue_upto(j):
        while state["issued"] <= min(j, len(jobs) - 1):
            i = state["issued"]
            s = slots[i % NS]
            for (c0, c1, src) in jobs[i]:
                fw.dma("pool", s[:, :, c0:c1], src, writes=[s], dname=f"w{i % NS}")
            state["issued"] += 1

    def wsrc(mat, c0, c1):
        return mat[:, c0:c1].rearrange("(k p) n -> p k n", p=128)

    jidx = {}

    def addjob(key, parts):
        jidx[key] = len(jobs)
        jobs.append(parts)

    for l in range(L):
        for j in range(6):
            addjob(("ada", l, j), [(0, 1024, wsrc(adaw_d[l], j * 1024, (j + 1) * 1024))])
    for l in range(L):
        for g in range(NG):
            for c in range(KC):
                addjob(("s1", l, g, c), [(jj * 128, (jj + 1) * 128, wsrc(win_d[l], (2 + jj) * 1024 + c * 128, (2 + jj) * 1024 + (c + 1) * 128)) for jj in range(3)])
            addjob(("wpb", l, g), [(0, 1024, wsrc(wpb_d[l], 0, 1024))])
            addjob(("zgb", l, g), [(0, 1024, wsrc(win_d[l], 6144, 7168))])
            addjob(("wo1", l, g), [(0, 1024, wsrc(wo_d[l], 0, 1024))])
            for c in range(KC):
                addjob(("s3", l, g, c), [(jj * 128, (jj + 1) * 128, wsrc(win_d[l], jj * 1024 + c * 128, jj * 1024 + (c + 1) * 128)) for jj in range(2)])
            addjob(("wpa", l, g), [(0, 1024, wsrc(wpa_d[l], 0, 1024))])
            addjob(("zga", l, g), [(0, 1024, wsrc(win_d[l], 5120, 6144))])
            addjob(("wo2", l, g), [(0, 1024, wsrc(wo_d[l], 0, 1024))])
            for e in range(NEXP):
                addjob(("w1a", l, g, e), [(0, 512, wsrc(w1_d[l, e], 0, 512)), (512, 1024, wsrc(w1_d[l, e], 1024, 1536))])
                addjob(("w1b", l, g, e), [(0, 512, wsrc(w1_d[l, e], 512, 1024)), (512, 1024, wsrc(w1_d[l, e], 1536, 2048))])
                addjob(("w2", l, g, e), [(0, 1024, wsrc(w2_d[l, e], 0, 1024))])

    def getw(key, first=None):
        j = jidx[key]
        issue_upto(jidx[first if first is not None else key] + NS - 1)
        return slots[j % NS]

    def mm(out_ap, outbuf, pairs, reads):
        n = len(pairs)
        for i, (lt, rh) in enumerate(pairs):
            fw.op("pe", lambda: PE.matmul(out_ap, lhsT=lt, rhs=rh, start=(i == 0), stop=(i == n - 1)),
                  reads, [outbuf], mark=(i == n - 1))

    def col(j):
        return cols[:, j:j + 1]

    SETS = [dict(mean=meanT, rstd=rstdT, msq=msqT), dict(mean=meanT2, rstd=rstdT2, msq=msqT2)]

    def ln_stats(srcs, srcbufs, n, bf_sum=False, si=0):
        st = SETS[si]
        mT, rT, qT = st["mean"], st["rstd"], st["msq"]
        p1, p2 = (pS1, pS2) if si == 0 else (prot.next(), prot.next())
        if bf_sum:
            mm(p1[:, 0:n], p1, [(ones_b[:, :], s_) for s_ in srcs], srcbufs + [ones_b])
        else:
            mm(p1[:, 0:n], p1, [(ones_f[:, :], s_) for s_ in srcs], srcbufs + [ones_f])
        for c in range(KC):
            sq = tmpA.next()
            fw.op("act", lambda: A.activation(out=sq[:, 0:n], in_=srcs[c], func=AF.Square), srcbufs, [sq])
            fw.op("pe", lambda: PE.matmul(p2[:, 0:n], lhsT=ones_f[:, :], rhs=sq[:, 0:n], start=(c == 0), stop=(c == KC - 1)),
                  [sq, ones_f], [p2], mark=True)
        fw.op("dve", lambda: V.tensor_scalar(out=mT[:, 0:n], in0=p1[:, 0:n], scalar1=1.0 / D, scalar2=None, op0=ALU.mult), [p1], [mT])
        fw.op("dve", lambda: V.tensor_tensor(out=qT[:, 0:n], in0=mT[:, 0:n], in1=mT[:, 0:n], op=ALU.mult), [mT], [qT])
        fw.op("dve", lambda: V.scalar_tensor_tensor(out=qT[:, 0:n], in0=p2[:, 0:n], scalar=1.0 / D, in1=qT[:, 0:n], op0=ALU.mult, op1=ALU.subtract), [p2, qT], [qT])
        fw.op("dve", lambda: V.tensor_scalar(out=qT[:, 0:n], in0=qT[:, 0:n], scalar1=0.0, scalar2=None, op0=ALU.max), [qT], [qT])
        fw.op("act", lambda: A.activation(out=qT[:, 0:n], in_=qT[:, 0:n], func=AF.Sqrt, bias=epsb[:, 0:1], scale=1.0), [qT, epsb], [qT])
        fw.op("dve", lambda: V.reciprocal(out=rT[:, 0:n], in_=qT[:, 0:n]), [qT], [rT])

    def ln_apply(src_ap, srcbuf, n, out_ap, outbuf, scale_ap, bias_ap, scbufs, func=AF.Identity, si=0):
        st = SETS[si]
        mT, rT = st["mean"], st["rstd"]
        t = tmpD.next()
        fw.op("dve", lambda: V.tensor_tensor(out=t[:, 0:n], in0=src_ap, in1=mT[:, 0:n], op=ALU.subtract), [srcbuf, mT], [t])
        fw.op("dve", lambda: V.tensor_tensor(out=t[:, 0:n], in0=t[:, 0:n], in1=rT[:, 0:n], op=ALU.mult), [t, rT], [t])
        fw.op("act", lambda: A.activation(out=out_ap, in_=t[:, 0:n], func=func, bias=bias_ap, scale=scale_ap), [t] + scbufs, [outbuf])

    fw.dma("sp", xs[:, :, :], xT_d, writes=[xs], dname="in")
    fw.dma("sp", ident[:, :], ident_d, writes=[ident], dname="ident")
    fw.dma("sp", maskt[:, :], mask_d, writes=[maskt], dname="mask")
    fw.dma("sp", condf[:, :], cT_d, writes=[condf], dname="cond")
    fw.op("dve", lambda: V.memset(ones_f[:, :], 1.0), [], [ones_f])
    fw.op("dve", lambda: V.memset(ones_b[:, :], 1.0), [], [ones_b])
    fw.op("dve", lambda: V.tensor_copy(out=ident_b[:, :], in_=ident[:, :]), [ident], [ident_b])
    fw.op("dve", lambda: V.memset(epsb[:, :], EPS), [], [epsb])
    fw.op("dve", lambda: V.memset(c119[:, :], 1.702 * 7.0), [], [c119])
    fw.op("dve", lambda: V.memset(c7[:, :], 7.0), [], [c7])
    fw.op("act", lambda: A.activation(out=condb[:, :], in_=condf[:, :], func=AF.Silu), [condf], [condb])

    for l in range(L):
        fw.dma("sp", cols[:, :], cols_d[l], writes=[cols], dname="cols")
        pm = pS1
        for j in range(6):
            s = getw(("ada", l, j))
            for m in range(8):
                mm(pm[:, j * 8 + m: j * 8 + m + 1], pm,
                   [(s[:, k, m * 128:(m + 1) * 128], condb[:, k:k + 1]) for k in range(KC)], [s, condb])
        fw.op("dve", lambda: V.tensor_tensor(out=modT[:, l * 48:(l + 1) * 48], in0=pm[:, 0:48], in1=cols[:, C_ADB:C_ADB + 48], op=ALU.add), [pm, cols], [modT])
        fw.op("dve", lambda: V.tensor_scalar(out=op1[:, l * 16:l * 16 + 8], in0=modT[:, l * 48 + 8:l * 48 + 16], scalar1=1.0, scalar2=None, op0=ALU.add), [modT], [op1])
        fw.op("dve", lambda: V.tensor_scalar(out=op1[:, l * 16 + 8:l * 16 + 16], in0=modT[:, l * 48 + 32:l * 48 + 40], scalar1=1.0, scalar2=None, op0=ALU.add), [modT], [op1])

    def md(l, j):
        return modT[:, l * 48 + j: l * 48 + j + 1]

    for l in range(L):
        if not (L == 1):
            fw.dma("sp", cols[:, :], cols_d[l], writes=[cols], dname="cols")
        elif l > 0:
            pass
        fw.op("dve", lambda: V.tensor_scalar(out=b1s[:, :], in0=cols[:, C_B1:C_B1 + NE * 16], scalar1=1.0, scalar2=None, op0=ALU.add), [cols], [b1s])
        fw.op("dve", lambda: V.tensor_scalar(out=b1s[:, :].rearrange("p (e j) -> p e j", j=16)[:, :, 0:8],
                                             in0=cols[:, C_B1:C_B1 + NE * 16].rearrange("p (e j) -> p e j", j=16)[:, :, 0:8],
                                             scalar1=-1.0, scalar2=7.0, op0=ALU.mult, op1=ALU.add), [cols], [b1s])
        fw.dma("pool", rwb[:, :, :], rw_d[l].rearrange("(k p) n -> p k n", p=128), writes=[rwb], dname="rwb")
        fw.dma("pool", rbb[:, :], rb_d[l:l + 1, :], writes=[rbb], dname="rbb")

        for g in range(NG):
            g0 = g * GT
            gblocks = blocks_of(GT)
            def xsrc(o, n):
                return [xs[:, c, g0 + o:g0 + o + n] for c in range(KC)]
            ln_stats(xsrc(*gblocks[0]), [xs], gblocks[0][1], si=0)
            for bi, (o, n) in enumerate(gblocks):
                if bi + 1 < len(gblocks):
                    ln_stats(xsrc(*gblocks[bi + 1]), [xs], gblocks[bi + 1][1], si=(bi + 1) % 2)
                srcs = xsrc(o, n)
                for c in range(KC):
                    ln_apply(srcs[c], xs, n, uT[:, c, o:o + n], uT, op1[:, l * 16 + c:l * 16 + c + 1], md(l, c), [op1, modT], si=bi % 2)
                for c in range(KC):
                    fw.op("act", lambda: A.mul(out=srcs[c], in_=srcs[c], mul=ALPHA), [xs], [xs])

            for c in (range(KC) if "skipB" not in DBG else []):
                s = getw(("s1", l, g, c))
                pp = ypad_v
                gb_t = gbt_v
                if g == 0:
                    fw.op("dve", lambda: V.memset(pp[:, 0:2], 0.0), [], [arena])
                else:
                    fw.op("dve", lambda: V.tensor_copy(out=pp[:, 0:2], in_=histB[:, c, :]), [histB], [arena])
                for (o, n) in gblocks:
                    pgb, pgc, phb = prot.next(), prot.next(), prot.next()
                    rhs = [uT[:, k, o:o + n] for k in range(KC)]
                    mm(pgb[:, 0:n], pgb, [(s[:, k, 0:128], rhs[k]) for k in range(KC)], [s, uT])
                    mm(pgc[:, 0:n], pgc, [(s[:, k, 128:256], rhs[k]) for k in range(KC)], [s, uT])
                    mm(phb[:, 0:n], phb, [(s[:, k, 256:384], rhs[k]) for k in range(KC)], [s, uT])
                    hbv = tmpA.next()
                    fw.op("act", lambda: A.activation(out=hbv[:, 0:n], in_=phb[:, 0:n], func=AF.Identity, bias=col(C_BIN + 32 + c), scale=1.0), [phb, cols], [hbv])
                    fw.op("dve", lambda: V.scalar_tensor_tensor(out=pp[:, 2 + o:2 + o + n], in0=pgc[:, 0:n], scalar=col(C_BIN + 24 + c), in1=hbv[:, 0:n], op0=ALU.add, op1=ALU.mult), [pgc, cols, hbv], [arena])
                    fw.op("act", lambda: A.activation(out=gb_t[:, o:o + n], in_=pgb[:, 0:n], func=AF.Identity, bias=col(C_BIN + 16 + c), scale=1.0), [pgb, cols], [arena])
                if g == 0:
                    fw.op("dve", lambda: V.tensor_tensor(out=pp[:, 2:2 + HALO], in0=pp[:, 2:2 + HALO], in1=maskt[:, :], op=ALU.mult), [arena, maskt], [arena])
                fw.op("dve", lambda: V.tensor_copy(out=histB[:, c, :], in_=pp[:, GT:GT + 2]), [arena], [histB])
                a = acc_v
                cw = C_CBW + c * 3
                fw.op("dve", lambda: V.tensor_scalar(out=a, in0=pp[:, 0:GT], scalar1=col(cw), scalar2=None, op0=ALU.mult), [arena, cols], [arena])
                fw.op("dve", lambda: V.scalar_tensor_tensor(out=a, in0=pp[:, 1:1 + GT], scalar=col(cw + 1), in1=a, op0=ALU.mult, op1=ALU.add), [arena, cols], [arena])
                fw.op("dve", lambda: V.scalar_tensor_tensor(out=a, in0=pp[:, 2:2 + GT], scalar=col(cw + 2), in1=a, op0=ALU.mult, op1=ALU.add), [arena, cols], [arena])
                fw.op("dve", lambda: V.tensor_tensor(out=yab[:, c, :], in0=a, in1=gb_t, op=ALU.mult), [arena], yabB)

            s_pb = getw(("wpb", l, g))
            s_zg = getw(("zgb", l, g), first=("wpb", l, g))
            s_wo = getw(("wo1", l, g), first=("wpb", l, g))
            for bi, (o, n) in enumerate(gblocks if "skipB" not in DBG else []):
                for c in range(KC):
                    py, pz = prot.next(), prot.next()
                    mm(py[:, 0:n], py, [(s_pb[:, k, c * 128:(c + 1) * 128], yab[:, k, o:o + n]) for k in range(KC)], [s_pb, yabB[bi]])
                    mm(pz[:, 0:n], pz, [(s_zg[:, k, c * 128:(c + 1) * 128], uT[:, k, o:o + n]) for k in range(KC)], [s_zg, uT])
                    sg = tmpA.next()
                    fw.op("act", lambda: A.activation(out=sg[:, 0:n], in_=pz[:, 0:n], func=AF.Sigmoid, bias=col(C_BIN + 48 + c), scale=1.0), [pz, cols], [sg])
                    fw.op("dve", lambda: V.tensor_tensor(out=mb_v[:, c, 0:n], in0=py[:, 0:n], in1=sg[:, 0:n], op=ALU.mult), [py, sg], [arena])
                for c in range(KC):
                    po = prot.next()
                    mm(po[:, 0:n], po, [(s_wo[:, k, c * 128:(c + 1) * 128], mb_v[:, k, 0:n]) for k in range(KC)], [s_wo, arena])
                    xa = xs[:, c, g0 + o:g0 + o + n]
                    fw.op("dve", lambda: V.scalar_tensor_tensor(out=xa, in0=po[:, 0:n], scalar=md(l, 16 + c), in1=xa, op0=ALU.mult, op1=ALU.add), [po, modT, xs], [xs])

            for c in (range(KC) if "skipA" not in DBG else []):
                s = getw(("s3", l, g, c))
                yp = ypb_v
                cw = C_CAW + c * 31
                for k in range(31):
                    fw.op("dve", lambda: V.tensor_scalar(out=dg_v[:, k, :], in0=ident_b[:, :], scalar1=col(cw + k), scalar2=None, op0=ALU.mult), [ident_b, cols], [arena])
                if g == 0:
                    fw.op("dve", lambda: V.memset(yp[:, 0:30], 0.0), [], [arena])
                else:
                    fw.op("dve", lambda: V.tensor_copy(out=yp[:, 0:30], in_=histA[:, c, :]), [histA], [arena])
                for (o, n) in gblocks:
                    pv, pg = prot.next(), prot.next()
                    rhs = [uT[:, k, o:o + n] for k in range(KC)]
                    mm(pv[:, 0:n], pv, [(s[:, k, 0:128], rhs[k]) for k in range(KC)], [s, uT])
                    mm(pg[:, 0:n], pg, [(s[:, k, 128:256], rhs[k]) for k in range(KC)], [s, uT])
                    sg = tmpA.next()
                    fw.op("act", lambda: A.activation(out=sg[:, 0:n], in_=pg[:, 0:n], func=AF.Sigmoid, bias=col(C_BIN + 8 + c), scale=1.0), [pg, cols], [sg])
                    fw.op("dve", lambda: V.scalar_tensor_tensor(out=yp[:, 30 + o:30 + o + n], in0=pv[:, 0:n], scalar=col(C_BIN + c), in1=sg[:, 0:n], op0=ALU.add, op1=ALU.mult), [pv, cols, sg], [arena])
                if g == 0:
                    fw.op("dve", lambda: V.tensor_tensor(out=yp[:, 30:30 + HALO], in0=yp[:, 30:30 + HALO], in1=maskt[:, :], op=ALU.mult), [arena, maskt], [arena])
                fw.op("dve", lambda: V.tensor_copy(out=histA[:, c, :], in_=yp[:, GT:GT + 30]), [arena], [histA])
                for bi, (o, n) in enumerate(gblocks):
                    pc = prot.next()
                    mm(pc[:, 0:n], pc, [(dg_v[:, k, :], yp[:, o + k:o + k + n]) for k in range(31)], [arena])
                    fw.op("act", lambda: A.activation(out=yab[:, c, o:o + n], in_=pc[:, 0:n], func=AF.Identity, bias=col(C_CAB + c), scale=1.0), [pc, cols], [yabB[bi]])

            s_pa = getw(("wpa", l, g))
            s_zg = getw(("zga", l, g), first=("wpa", l, g))
            s_wo = getw(("wo2", l, g), first=("wpa", l, g))
            def ysrc(o, n):
                return [yab[:, c, o:o + n] for c in range(KC)]
            if "skipA" not in DBG:
                ln_stats(ysrc(*gblocks[0]), [yabB[0]], gblocks[0][1], bf_sum=True, si=0)
            for bi, (o, n) in enumerate(gblocks if "skipA" not in DBG else []):
                if bi + 1 < len(gblocks):
                    ln_stats(ysrc(*gblocks[bi + 1]), [yabB[bi + 1]], gblocks[bi + 1][1], bf_sum=True, si=(bi + 1) % 2)
                srcs = ysrc(o, n)
                for c in range(KC):
                    ln_apply(srcs[c], yabB[bi], n, sbf_v[:, c, 0:n], arena, col(C_LAG + c), col(C_LAB + c), [cols], func=AF.Silu, si=bi % 2)
                for c in range(KC):
                    py, pz = prot.next(), prot.next()
                    mm(py[:, 0:n], py, [(s_pa[:, k, c * 128:(c + 1) * 128], sbf_v[:, k, 0:n]) for k in range(KC)], [s_pa, arena])
                    mm(pz[:, 0:n], pz, [(s_zg[:, k, c * 128:(c + 1) * 128], uT[:, k, o:o + n]) for k in range(KC)], [s_zg, uT])
                    sg = tmpA.next()
                    fw.op("act", lambda: A.activation(out=sg[:, 0:n], in_=pz[:, 0:n], func=AF.Sigmoid, bias=col(C_BIN + 40 + c), scale=1.0), [pz, cols], [sg])
                    fw.op("dve", lambda: V.scalar_tensor_tensor(out=mb_v[:, c, 0:n], in0=py[:, 0:n], scalar=col(C_BPA + c), in1=sg[:, 0:n], op0=ALU.add, op1=ALU.mult), [py, cols, sg], [arena])
                for c in range(KC):
                    po = prot.next()
                    mm(po[:, 0:n], po, [(s_wo[:, k, c * 128:(c + 1) * 128], mb_v[:, k, 0:n]) for k in range(KC)], [s_wo, arena])
                    xa = xs[:, c, g0 + o:g0 + o + n]
                    fw.op("dve", lambda: V.scalar_tensor_tensor(out=xa, in0=po[:, 0:n], scalar=md(l, 16 + c), in1=xa, op0=ALU.mult, op1=ALU.add), [po, modT, xs], [xs])

            ln_stats(xsrc(*gblocks[0]), [xs], gblocks[0][1], si=0)
            for bi, (o, n) in enumerate(gblocks):
                srcs = xsrc(o, n)
                si = bi % 2
                for c in range(KC):
                    ln_apply(srcs[c], xs, n, srcs[c], xs, col(C_L1G + c), col(C_L1B + c), [cols], si=si)
                if bi + 1 < len(gblocks):
                    ln_stats(xsrc(*gblocks[bi + 1]), [xs], gblocks[bi + 1][1], si=(bi + 1) % 2)
                ln_stats(srcs, [xs], n, si=si)
                for c in range(KC):
                    ln_apply(srcs[c], xs, n, uT[:, c, o:o + n], uT, op1[:, l * 16 + 8 + c:l * 16 + 9 + c], md(l, 24 + c), [op1, modT], si=si)
                for c in range(KC):
                    fw.op("act", lambda: A.mul(out=srcs[c], in_=srcs[c], mul=ALPHA), [xs], [xs])

            fw.dma("sp", b2t_v, b2_d[l], writes=[arena], dname="b2t")
            for o in range(0, GT, 128):
                m = min(128, GT - o)
                pl = pS1
                pairs = [(uT[:, k, o:o + m], rwb[:, k, :]) for k in range(KC)] + [(ones_b[0:1, 0:m], rbb[0:1, :])]
                mm(pl[0:m, 0:NE], pl, pairs, [uT, rwb, ones_b, rbb])
                sm = small.next()
                sm2 = small.next()
                lg, msk, m8, sc = sm[0:m, 0:32], sm[0:m, 32:64], sm[0:m, 64:72], sm[0:m, 72:80]
                ex, em, gt_ = sm2[0:m, 0:32], sm2[0:m, 32:64], sm2[0:m, 64:96]
                fw.op("act", lambda: A.copy(out=lg, in_=pl[0:m, 0:NE]), [pl], [sm])
                fw.op("dve", lambda: V.max(out=m8, in_=lg), [sm], [sm])
                fw.op("dve", lambda: V.tensor_scalar(out=msk, in0=lg, scalar1=m8[:, 3:4], scalar2=None, op0=ALU.is_ge), [sm], [sm])
                fw.op("dve", lambda: V.tensor_scalar(out=sc[:, 0:1], in0=m8[:, 0:1], scalar1=-1.0, scalar2=None, op0=ALU.mult), [sm], [sm])
                fw.op("act", lambda: A.activation(out=ex, in_=lg, func=AF.Exp, bias=sc[:, 0:1], scale=1.0), [sm], [sm2])
                fw.op("dve", lambda: V.tensor_tensor(out=em, in0=ex, in1=msk, op=ALU.mult), [sm, sm2], [sm2])
                fw.op("dve", lambda: V.reduce_sum(out=sc[:, 1:2], in_=em, axis=mybir.AxisListType.X), [sm2], [sm])
                fw.op("dve", lambda: V.reciprocal(out=sc[:, 2:3], in_=sc[:, 1:2]), [sm], [sm])
                fw.op("dve", lambda: V.tensor_scalar(out=gt_, in0=em, scalar1=sc[:, 2:3], scalar2=None, op0=ALU.mult), [sm, sm2], [sm2])
                pt = pS2
                mm(pt[0:NE, 0:m], pt, [(gt_, ident[0:m, 0:m])], [sm2, ident])
                fw.op("act", lambda: A.copy(out=gateT_v[:, o:o + m], in_=pt[0:NE, 0:m]), [pt], [arena])

            for (o, n) in (gblocks if NEXP > 0 else []):
                for c in range(KC):
                    po = prot.next()
                    mm(po[:, 0:n], po, [(b2t_v[:, c * 128:(c + 1) * 128], gateT_v[:, o:o + n])], [arena])
                    xa = xs[:, c, g0 + o:g0 + o + n]
                    fw.op("dve", lambda: V.scalar_tensor_tensor(out=xa, in0=po[:, 0:n], scalar=md(l, 40 + c), in1=xa, op0=ALU.mult, op1=ALU.add), [po, modT, xs], [xs])
            pend = []
            def emit_ge(e):
                for bi, (o, n) in enumerate(gblocks):
                    pge = pS1 if bi % 2 == 0 else pS2
                    mm(pge[:, 0:n], pge, [(ident[0:NE, e:e + 1].to_broadcast([NE, 128]), gateT_v[:, o:o + n])], [ident, arena])
                    fw.op("act", lambda: A.copy(out=ge_v[bi][:, 0:n], in_=pge[:, 0:n]), [pge], [arena])

            if NEXP > 0:
                emit_ge(0)
            for e in range(NEXP):
                for half, key in ((0, "w1a"), (1, "w1b")):
                    s = getw((key, l, g, e))
                    for bi, (o, n) in enumerate(gblocks):
                        gs = ge_v[bi]
                        for hh in range(4):
                            hc = half * 4 + hh
                            pg_, pl_ = prot.next(), prot.next()
                            rhs = [uT[:, k, o:o + n] for k in range(KC)]
                            mm(pg_[:, 0:n], pg_, [(s[:, k, hh * 128:(hh + 1) * 128], rhs[k]) for k in range(KC)], [s, uT])
                            mm(pl_[:, 0:n], pl_, [(s[:, k, 512 + hh * 128:512 + (hh + 1) * 128], rhs[k]) for k in range(KC)], [s, uT])
                            r2 = tmpD.next()
                            fw.op("act", lambda: A.activation(out=r2[:, 0:n], in_=pg_[:, 0:n], func=AF.Relu, bias=b1s[:, e * 16 + hc:e * 16 + hc + 1], scale=-1.0), [pg_, b1s], [r2])
                            sg = tmpA.next()
                            fw.op("act", lambda: A.activation(out=sg[:, 0:n], in_=r2[:, 0:n], func=AF.Sigmoid, bias=c119[:, 0:1], scale=-1.702), [r2, c119], [sg])
                            lin = tmpL.next()
                            fw.op("dve", lambda: V.tensor_scalar(out=lin[:, 0:n], in0=pl_[:, 0:n], scalar1=b1s[:, e * 16 + 8 + hc:e * 16 + 9 + hc], scalar2=-6.0, op0=ALU.add, op1=ALU.max), [pl_, b1s], [lin])
                            fw.op("dve", lambda: V.scalar_tensor_tensor(out=lin[:, 0:n], in0=lin[:, 0:n], scalar=8.0, in1=gs[:, 0:n], op0=ALU.min, op1=ALU.mult), [lin, arena], [lin])
                            fw.op("act", lambda: A.activation(out=r2[:, 0:n], in_=r2[:, 0:n], func=AF.Identity, bias=c7[:, 0:1], scale=-1.0), [r2, c7], [r2])
                            tp = tmpP.next()
                            fw.op("pool", lambda: G.tensor_tensor(out=tp[:, 0:n], in0=r2[:, 0:n], in1=sg[:, 0:n], op=ALU.mult), [r2, sg], [tp])
                            if pend:
                                pend.pop()()
                            if hc % 2 == 0:
                                fw.op("pool", lambda: G.tensor_tensor(out=yab[:, hc, o:o + n], in0=tp[:, 0:n], in1=lin[:, 0:n], op=ALU.mult), [tp, lin], [yabB[bi]])
                            else:
                                def fin(tp=tp, lin=lin, hc=hc, o=o, n=n, bi=bi):
                                    fw.op("dve", lambda: V.tensor_tensor(out=yab[:, hc, o:o + n], in0=tp[:, 0:n], in1=lin[:, 0:n], op=ALU.mult), [tp, lin], [yabB[bi]])
                                pend.append(fin)
                if pend:
                    pend.pop()()
                if e + 1 < NEXP:
                    emit_ge(e + 1)
                s = getw(("w2", l, g, e))
                for bi, (o, n) in enumerate(gblocks):
                    for c in range(KC):
                        po = prot.next()
                        mm(po[:, 0:n], po, [(s[:, k, c * 128:(c + 1) * 128], yab[:, k, o:o + n]) for k in range(KC)], [s, yabB[bi]])
                        xa = xs[:, c, g0 + o:g0 + o + n]
                        fw.op("dve", lambda: V.scalar_tensor_tensor(out=xa, in0=po[:, 0:n], scalar=md(l, 40 + c), in1=xa, op0=ALU.mult, op1=ALU.add), [po, modT, xs], [xs])

            ln_stats(xsrc(*gblocks[0]), [xs], gblocks[0][1], si=0)
            for bi, (o, n) in enumerate(gblocks):
                if bi + 1 < len(gblocks):
                    ln_stats(xsrc(*gblocks[bi + 1]), [xs], gblocks[bi + 1][1], si=(bi + 1) % 2)
                srcs = xsrc(o, n)
                for c in range(KC):
                    ln_apply(srcs[c], xs, n, srcs[c], xs, col(C_L2G + c), col(C_L2B + c), [cols], si=bi % 2)

    d = fw.dma("sp", yT_d, xs[:, :, HALO:T], reads=[xs], dname="out")
    fw._wait("sp", d)
    fw.close()
    return nc


def _pack_cols(inp, L):
    cols = np.zeros((L, 128, NCOL), np.float32)

    def put(l, c0, vec):
        v = np.asarray(vec, np.float32).reshape(-1, 128).T
        cols[l, :, c0:c0 + v.shape[1]] = v

    for l in range(L):
        put(l, C_BIN, inp["b_in"][l])
        caw = np.asarray(inp["conv_a_w"][l], np.float32)
        cols[l, :, C_CAW:C_CAW + 248] = caw.reshape(31, 8, 128).transpose(2, 1, 0).reshape(128, 248)
        put(l, C_CAB, inp["conv_a_b"][l])
        put(l, C_LAG, inp["ln_a_g"][l])
        put(l, C_LAB, inp["ln_a_b"][l])
        cbw = np.asarray(inp["conv_b_w"][l], np.float32)
        cols[l, :, C_CBW:C_CBW + 24] = cbw.reshape(3, 8, 128).transpose(2, 1, 0).reshape(128, 24)
        put(l, C_BPA, inp["b_pa"][l])
        put(l, C_L1G, inp["ln1_g"][l])
        put(l, C_L1B, inp["ln1_b"][l])
        put(l, C_L2G, inp["ln2_g"][l])
        put(l, C_L2B, inp["ln2_b"][l])
        put(l, C_ADB, inp["ada_b"][l])
        put(l, C_B1, np.asarray(inp["b1"][l], np.float32).reshape(-1))
    return cols


_CACHE = {}


def kernel(**inp):
    L = DEPTH
    B, S, _ = inp["x"].shape
    NCORE = 8
    per = (B * S) // NCORE
    cps = S // per
    T = per + HALO
    GT = T // 2
    key = (L, T, GT)
    if key not in _CACHE:
        _CACHE[key] = build_program(L, T, GT)
    nc = _CACHE[key]
    x = np.asarray(inp["x"], np.float32)
    cols = _pack_cols(inp, L)
    ident = np.eye(128, dtype=np.float32)
    shared = dict(
        ident=ident, cols=cols,
        ada_w=np.ascontiguousarray(inp["ada_w"], np.float32), w_in=np.ascontiguousarray(inp["w_in"], np.float32),
        w_pa=np.ascontiguousarray(inp["w_pa"], np.float32), w_pb=np.ascontiguousarray(inp["w_pb"], np.float32),
        w_o=np.ascontiguousarray(inp["w_o"], np.float32), router_w=np.ascontiguousarray(inp["router_w"], np.float32),
        router_b=np.ascontiguousarray(inp["router_b"], np.float32), w1=np.ascontiguousarray(inp["w1"], np.float32),
        w2=np.ascontiguousarray(inp["w2"], np.float32), b2=np.ascontiguousarray(inp["b2"], np.float32),
    )
    in_maps = []
    for core in range(NCORE):
        b, q = divmod(core, cps)
        s0 = q * per
        xt = np.zeros((T, D), np.float32)
        if q == 0:
            xt[HALO:] = x[b, 0:per]
            mask = np.zeros((128, 128), np.float32)
        else:
            xt[:] = x[b, s0 - HALO:s0 + per]
            mask = np.ones((128, 128), np.float32)
        xT = np.ascontiguousarray(xt.T.reshape(KC, 128, T).transpose(1, 0, 2))
        cT = np.ascontiguousarray(np.asarray(inp["c"], np.float32)[b].reshape(KC, 128).T)
        m = dict(shared)
        m.update(xT=xT, cT=cT, mask=mask)
        in_maps.append(m)
    res = run_bass_kernel_spmd(nc, in_maps, core_ids=list(range(NCORE)))
    out = np.zeros((B, S, D), np.float32)
    for core in range(NCORE):
        b, q = divmod(core, cps)
        yT = np.asarray(res.results[core]["yT"])
        out[b, q * per:(q + 1) * per, :] = yT.transpose(2, 1, 0).reshape(per, D)
    return out
```

```python
from contextlib import ExitStack
import numpy as np
import concourse.bass as bass
import concourse.mybir as mybir
from concourse.bass_utils import run_bass_kernel_spmd

F32 = mybir.dt.float32
BF16 = mybir.dt.bfloat16
AF = mybir.ActivationFunctionType
ALU = mybir.AluOpType

D = 1024
KC = 8
NE = 32
DEPTH = 4
ALPHA = (2.0 * DEPTH) ** 0.25
EPS = 1e-5
HALO = 128
N_IN = 7168

C_BIN = 0
C_CAW = 56
C_CAB = 304
C_LAG = 312
C_LAB = 320
C_CBW = 328
C_BPA = 352
C_L1G = 360
C_L1B = 368
C_L2G = 376
C_L2B = 384
C_ADB = 392
C_B1 = 440
NCOL = 952

ENGS = ("pe", "act", "dve", "pool", "sp")


class Buf:
    def __init__(self, name, handle, excl=False):
        self.excl = excl
        self.name = name
        self.h = handle
        self.last_w = None
        self.readers = []

    def __getitem__(self, k):
        return self.h[k]


class FW:
    def __init__(self, nc):
        self.nc = nc
        self.es = ExitStack()
        self.E = {"pe": nc.tensor, "act": nc.scalar, "dve": nc.vector, "pool": nc.gpsimd, "sp": nc.sync}
        self.sem = {e: self.es.enter_context(nc.semaphore("s_" + e)) for e in ENGS}
        self.tick = {e: 0 for e in ENGS}
        self.seen = {e: {} for e in ENGS}
        self.dsems = {}
        self.dcnt = {}

    def sbuf(self, name, shape, dt):
        h = self.es.enter_context(self.nc.sbuf_tensor(name, list(shape), dt))
        return Buf(name, h)

    def psum(self, name, shape, dt):
        h = self.es.enter_context(self.nc.psum_tensor(name, list(shape), dt))
        return Buf(name, h, excl=True)

    def dsem(self, name):
        self.dsems[name] = self.es.enter_context(self.nc.semaphore("d_" + name))
        self.dcnt[name] = 0
        return name

    def _wait(self, e, dep):
        kind, key, tick = dep
        if kind == "e" and key == e and e == "pe":
            return
        sem = self.sem[key] if kind == "e" else self.dsems[key]
        sk = (kind, key)
        if self.seen[e].get(sk, 0) >= tick:
            return
        self.E[e].wait_ge(sem, tick)
        self.seen[e][sk] = tick

    def deps(self, e, reads, writes):
        for b in reads:
            if b.last_w is not None:
                self._wait(e, b.last_w)
            if b.excl:
                for r in b.readers:
                    if not (r[0] == "e" and r[1] == e):
                        self._wait(e, r)
        for b in writes:
            if b.last_w is not None:
                self._wait(e, b.last_w)
            for r in b.readers:
                self._wait(e, r)

    def done(self, e, ins, reads, writes, mark=True):
        if mark:
            self.tick[e] += 1
            ins.then_inc(self.sem[e], 1)
            t = self.tick[e]
        else:
            t = self.tick[e] + 1
        d = ("e", e, t)
        for b in reads:
            b.readers = [r for r in b.readers if not (r[0] == "e" and r[1] == e)] + [d]
        for b in writes:
            b.last_w = d
            b.readers = []
        return ins

    def op(self, e, fn, reads, writes, mark=True):
        self.deps(e, reads, writes)
        ins = fn()
        return self.done(e, ins, reads, writes, mark)

    def dma(self, q, out_ap, in_ap, reads=(), writes=(), dname=None):
        self.deps(q, reads, writes)
        ins = self.E[q].dma_start(out=out_ap, in_=in_ap)
        self.dcnt[dname] += 16
        ins.then_inc(self.dsems[dname], 16)
        d = ("d", dname, self.dcnt[dname])
        for b in reads:
            b.readers = b.readers + [d]
        for b in writes:
            b.last_w = d
            b.readers = []
        return d

    def close(self):
        self.es.close()


class Rot:
    def __init__(self, bufs):
        self.bufs = bufs
        self.i = 0

    def next(self):
        b = self.bufs[self.i % len(self.bufs)]
        self.i += 1
        return b


def blocks_of(n):
    nb = (n + 511) // 512
    base = -(-n // nb)
    base = -(-base // 64) * 64
    out, o = [], 0
    while o < n:
        s = min(base, n - o)
        out.append((o, s))
        o += s
    return out


def build_program(L, T, GT, NEXP=NE, DBG=""):
    assert T % GT == 0 and GT % 64 == 0
    NG = T // GT
    TOWN = T - HALO
    nc = bass.Bass("TRN2", target_bir_lowering=False)
    dt_ = nc.dram_tensor
    xT_d = dt_("xT", [128, KC, T], F32, kind="ExternalInput").ap()
    cT_d = dt_("cT", [128, KC], F32, kind="ExternalInput").ap()
    mask_d = dt_("mask", [128, 128], F32, kind="ExternalInput").ap()
    ident_d = dt_("ident", [128, 128], F32, kind="ExternalInput").ap()
    cols_d = dt_("cols", [L, 128, NCOL], F32, kind="ExternalInput").ap()
    adaw_d = dt_("ada_w", [L, D, 6 * D], F32, kind="ExternalInput").ap()
    win_d = dt_("w_in", [L, D, N_IN], F32, kind="ExternalInput").ap()
    wpa_d = dt_("w_pa", [L, D, D], F32, kind="ExternalInput").ap()
    wpb_d = dt_("w_pb", [L, D, D], F32, kind="ExternalInput").ap()
    wo_d = dt_("w_o", [L, D, D], F32, kind="ExternalInput").ap()
    rw_d = dt_("router_w", [L, D, NE], F32, kind="ExternalInput").ap()
    rb_d = dt_("router_b", [L, NE], F32, kind="ExternalInput").ap()
    w1_d = dt_("w1", [L, NE, D, 2 * D], F32, kind="ExternalInput").ap()
    w2_d = dt_("w2", [L, NE, D, D], F32, kind="ExternalInput").ap()
    b2_d = dt_("b2", [L, NE, D], F32, kind="ExternalInput").ap()
    yT_d = dt_("yT", [128, KC, TOWN], F32, kind="ExternalOutput").ap()

    fw = FW(nc)
    V, A, G, PE = nc.vector, nc.scalar, nc.gpsimd, nc.tensor

    xs = fw.sbuf("xs", [128, KC, T], F32)
    uT = fw.sbuf("uT", [128, KC, GT], BF16)
    yab = fw.sbuf("yab", [128, KC, GT], BF16)
    yabB = [Buf(f"yab_b{i}", yab.h) for i in range(len(blocks_of(GT)))]
    NS = 3
    slots = [fw.sbuf(f"slot{i}", [128, KC, 1024], BF16) for i in range(NS)]
    arena = fw.sbuf("arena", [128, 4096], F32)
    assert 3 * GT + 30 <= 4096 and len(blocks_of(GT)) <= 3
    ypad_v = arena[:, 0:30 + GT]
    acc_v = arena[:, 30 + GT:30 + 2 * GT]
    gbt_v = arena[:, 30 + 2 * GT:30 + 3 * GT]
    YPW = (30 + GT) // 2
    ypb_v = arena[:, 0:YPW].bitcast(BF16)
    dg_v = arena[:, YPW:YPW + 31 * 64].bitcast(BF16).rearrange("p (k m) -> p k m", k=31)
    assert YPW + 31 * 64 <= 4096
    mb_v = arena[:, 0:2048].bitcast(BF16).rearrange("p (c n) -> p c n", c=KC)
    sbf_v = arena[:, 2048:4096].bitcast(BF16).rearrange("p (c n) -> p c n", c=KC)
    gateT_v = arena[0:NE, 0:GT]
    b2t_v = arena[0:NE, GT:GT + D]
    GE0 = GT + D
    ge_v = [arena[:, GE0 + i * 512:GE0 + (i + 1) * 512] for i in range(3)]
    assert GE0 + 3 * 512 <= 4096
    cols = fw.sbuf("cols_sb", [128, NCOL], F32)
    modT = fw.sbuf("modT", [128, L * 48], F32)
    op1 = fw.sbuf("op1", [128, L * 16], F32)
    b1s = fw.sbuf("b1s", [128, NE * 16], F32)
    ident = fw.sbuf("ident_sb", [128, 128], F32)
    maskt = fw.sbuf("maskt", [128, 128], F32)
    ones_f = fw.sbuf("ones_f", [128, 128], F32)
    ones_b = fw.sbuf("ones_b", [128, 128], BF16)
    epsb = fw.sbuf("epsb", [128, 1], F32)
    c119 = fw.sbuf("c119", [128, 1], F32)
    c7 = fw.sbuf("c7", [128, 1], F32)
    condf = fw.sbuf("condf", [128, KC], F32)
    condb = fw.sbuf("condb", [128, KC], BF16)
    histA = fw.sbuf("histA", [128, KC, 30], BF16)
    histB = fw.sbuf("histB", [128, KC, 2], F32)
    rwb = fw.sbuf("rwb", [128, KC, NE], BF16)
    rbb = fw.sbuf("rbb", [1, NE], BF16)
    tmpA = Rot([fw.sbuf(f"tmpA{i}", [128, 512], F32) for i in range(2)])
    tmpD = Rot([fw.sbuf(f"tmpD{i}", [128, 512], F32) for i in range(2)])
    tmpP = Rot([fw.sbuf(f"tmpP{i}", [128, 512], F32) for i in range(2)])
    tmpL = Rot([fw.sbuf(f"tmpL{i}", [128, 512], F32) for i in range(2)])
    meanT = fw.sbuf("meanT", [128, 512], F32)
    rstdT = fw.sbuf("rstdT", [128, 512], F32)
    msqT = fw.sbuf("msqT", [128, 512], F32)
    meanT2 = fw.sbuf("meanT2", [128, 512], F32)
    rstdT2 = fw.sbuf("rstdT2", [128, 512], F32)
    msqT2 = fw.sbuf("msqT2", [128, 512], F32)
    ident_b = fw.sbuf("ident_b", [128, 128], BF16)
    small = Rot([fw.sbuf(f"small{i}", [128, 96], F32) for i in range(2)])
    pb = [fw.psum(f"pb{i}", [128, 512], F32) for i in range(8)]
    prot = Rot(pb[0:6])
    pS1, pS2 = pb[6], pb[7]

    for n in ("in", "cols", "w0", "w1", "w2", "ident", "mask", "cond", "rwb", "rbb", "b2t", "out"):
        fw.dsem(n)

    jobs = []
    state = {"issued": 0}

    def issue_upto(j):
        while state["issued"] <= min(j, len(jobs) - 1):
            i = state["issued"]
            s = slots[i % NS]
            for (c0, c1, src) in jobs[i]:
                fw.dma("pool", s[:, :, c0:c1], src, writes=[s], dname=f"w{i % NS}")
            state["issued"] += 1

    def wsrc(mat, c0, c1):
        return mat[:, c0:c1].rearrange("(k p) n -> p k n", p=128)

    jidx = {}

    def addjob(key, parts):
        jidx[key] = len(jobs)
        jobs.append(parts)

    for l in range(L):
        for j in range(6):
            addjob(("ada", l, j), [(0, 1024, wsrc(adaw_d[l], j * 1024, (j + 1) * 1024))])
    for l in range(L):
        for g in range(NG):
            for c in range(KC):
                addjob(("s1", l, g, c), [(jj * 128, (jj + 1) * 128, wsrc(win_d[l], (2 + jj) * 1024 + c * 128, (2 + jj) * 1024 + (c + 1) * 128)) for jj in range(3)])
            addjob(("wpb", l, g), [(0, 1024, wsrc(wpb_d[l], 0, 1024))])
            addjob(("zgb", l, g), [(0, 1024, wsrc(win_d[l], 6144, 7168))])
            addjob(("wo1", l, g), [(0, 1024, wsrc(wo_d[l], 0, 1024))])
            for c in range(KC):
                addjob(("s3", l, g, c), [(jj * 128, (jj + 1) * 128, wsrc(win_d[l], jj * 1024 + c * 128, jj * 1024 + (c + 1) * 128)) for jj in range(2)])
            addjob(("wpa", l, g), [(0, 1024, wsrc(wpa_d[l], 0, 1024))])
            addjob(("zga", l, g), [(0, 1024, wsrc(win_d[l], 5120, 6144))])
            addjob(("wo2", l, g), [(0, 1024, wsrc(wo_d[l], 0, 1024))])
            for e in range(NEXP):
                addjob(("w1a", l, g, e), [(0, 512, wsrc(w1_d[l, e], 0, 512)), (512, 1024, wsrc(w1_d[l, e], 1024, 1536))])
                addjob(("w1b", l, g, e), [(0, 512, wsrc(w1_d[l, e], 512, 1024)), (512, 1024, wsrc(w1_d[l, e], 1536, 2048))])
                addjob(("w2", l, g, e), [(0, 1024, wsrc(w2_d[l, e], 0, 1024))])

    def getw(key, first=None):
        j = jidx[key]
        issue_upto(jidx[first if first is not None else key] + NS - 1)
        return slots[j % NS]

    def mm(out_ap, outbuf, pairs, reads):
        n = len(pairs)
        for i, (lt, rh) in enumerate(pairs):
            fw.op("pe", lambda: PE.matmul(out_ap, lhsT=lt, rhs=rh, start=(i == 0), stop=(i == n - 1)),
                  reads, [outbuf], mark=(i == n - 1))

    def col(j):
        return cols[:, j:j + 1]

    SETS = [dict(mean=meanT, rstd=rstdT, msq=msqT), dict(mean=meanT2, rstd=rstdT2, msq=msqT2)]

    def ln_stats(srcs, srcbufs, n, bf_sum=False, si=0):
        st = SETS[si]
        mT, rT, qT = st["mean"], st["rstd"], st["msq"]
        p1, p2 = (pS1, pS2) if si == 0 else (prot.next(), prot.next())
        if bf_sum:
            mm(p1[:, 0:n], p1, [(ones_b[:, :], s_) for s_ in srcs], srcbufs + [ones_b])
        else:
            mm(p1[:, 0:n], p1, [(ones_f[:, :], s_) for s_ in srcs], srcbufs + [ones_f])
        for c in range(KC):
            sq = tmpA.next()
            fw.op("act", lambda: A.activation(out=sq[:, 0:n], in_=srcs[c], func=AF.Square), srcbufs, [sq])
            fw.op("pe", lambda: PE.matmul(p2[:, 0:n], lhsT=ones_f[:, :], rhs=sq[:, 0:n], start=(c == 0), stop=(c == KC - 1)),
                  [sq, ones_f], [p2], mark=True)
        fw.op("dve", lambda: V.tensor_scalar(out=mT[:, 0:n], in0=p1[:, 0:n], scalar1=1.0 / D, scalar2=None, op0=ALU.mult), [p1], [mT])
        fw.op("dve", lambda: V.tensor_tensor(out=qT[:, 0:n], in0=mT[:, 0:n], in1=mT[:, 0:n], op=ALU.mult), [mT], [qT])
        fw.op("dve", lambda: V.scalar_tensor_tensor(out=qT[:, 0:n], in0=p2[:, 0:n], scalar=1.0 / D, in1=qT[:, 0:n], op0=ALU.mult, op1=ALU.subtract), [p2, qT], [qT])
        fw.op("dve", lambda: V.tensor_scalar(out=qT[:, 0:n], in0=qT[:, 0:n], scalar1=0.0, scalar2=None, op0=ALU.max), [qT], [qT])
        fw.op("act", lambda: A.activation(out=qT[:, 0:n], in_=qT[:, 0:n], func=AF.Sqrt, bias=epsb[:, 0:1], scale=1.0), [qT, epsb], [qT])
        fw.op("dve", lambda: V.reciprocal(out=rT[:, 0:n], in_=qT[:, 0:n]), [qT], [rT])

    def ln_apply(src_ap, srcbuf, n, out_ap, outbuf, scale_ap, bias_ap, scbufs, func=AF.Identity, si=0):
        st = SETS[si]
        mT, rT = st["mean"], st["rstd"]
        t = tmpD.next()
        fw.op("dve", lambda: V.tensor_tensor(out=t[:, 0:n], in0=src_ap, in1=mT[:, 0:n], op=ALU.subtract), [srcbuf, mT], [t])
        fw.op("dve", lambda: V.tensor_tensor(out=t[:, 0:n], in0=t[:, 0:n], in1=rT[:, 0:n], op=ALU.mult), [t, rT], [t])
        fw.op("act", lambda: A.activation(out=out_ap, in_=t[:, 0:n], func=func, bias=bias_ap, scale=scale_ap), [t] + scbufs, [outbuf])

    fw.dma("sp", xs[:, :, :], xT_d, writes=[xs], dname="in")
    fw.dma("sp", ident[:, :], ident_d, writes=[ident], dname="ident")
    fw.dma("sp", maskt[:, :], mask_d, writes=[maskt], dname="mask")
    fw.dma("sp", condf[:, :], cT_d, writes=[condf], dname="cond")
    fw.op("dve", lambda: V.memset(ones_f[:, :], 1.0), [], [ones_f])
    fw.op("dve", lambda: V.memset(ones_b[:, :], 1.0), [], [ones_b])
    fw.op("dve", lambda: V.tensor_copy(out=ident_b[:, :], in_=ident[:, :]), [ident], [ident_b])
    fw.op("dve", lambda: V.memset(epsb[:, :], EPS), [], [epsb])
    fw.op("dve", lambda: V.memset(c119[:, :], 1.702 * 7.0), [], [c119])
    fw.op("dve", lambda: V.memset(c7[:, :], 7.0), [], [c7])
    fw.op("act", lambda: A.activation(out=condb[:, :], in_=condf[:, :], func=AF.Silu), [condf], [condb])

    for l in range(L):
        fw.dma("sp", cols[:, :], cols_d[l], writes=[cols], dname="cols")
        pm = pS1
        for j in range(6):
            s = getw(("ada", l, j))
            for m in range(8):
                mm(pm[:, j * 8 + m: j * 8 + m + 1], pm,
                   [(s[:, k, m * 128:(m + 1) * 128], condb[:, k:k + 1]) for k in range(KC)], [s, condb])
        fw.op("dve", lambda: V.tensor_tensor(out=modT[:, l * 48:(l + 1) * 48], in0=pm[:, 0:48], in1=cols[:, C_ADB:C_ADB + 48], op=ALU.add), [pm, cols], [modT])
        fw.op("dve", lambda: V.tensor_scalar(out=op1[:, l * 16:l * 16 + 8], in0=modT[:, l * 48 + 8:l * 48 + 16], scalar1=1.0, scalar2=None, op0=ALU.add), [modT], [op1])
        fw.op("dve", lambda: V.tensor_scalar(out=op1[:, l * 16 + 8:l * 16 + 16], in0=modT[:, l * 48 + 32:l * 48 + 40], scalar1=1.0, scalar2=None, op0=ALU.add), [modT], [op1])

    def md(l, j):
        return modT[:, l * 48 + j: l * 48 + j + 1]

    for l in range(L):
        if not (L == 1):
            fw.dma("sp", cols[:, :], cols_d[l], writes=[cols], dname="cols")
        elif l > 0:
            pass
        fw.op("dve", lambda: V.tensor_scalar(out=b1s[:, :], in0=cols[:, C_B1:C_B1 + NE * 16], scalar1=1.0, scalar2=None, op0=ALU.add), [cols], [b1s])
        fw.op("dve", lambda: V.tensor_scalar(out=b1s[:, :].rearrange("p (e j) -> p e j", j=16)[:, :, 0:8],
                                             in0=cols[:, C_B1:C_B1 + NE * 16].rearrange("p (e j) -> p e j", j=16)[:, :, 0:8],
                                             scalar1=-1.0, scalar2=7.0, op0=ALU.mult, op1=ALU.add), [cols], [b1s])
        fw.dma("pool", rwb[:, :, :], rw_d[l].rearrange("(k p) n -> p k n", p=128), writes=[rwb], dname="rwb")
        fw.dma("pool", rbb[:, :], rb_d[l:l + 1, :], writes=[rbb], dname="rbb")

        for g in range(NG):
            g0 = g * GT
            gblocks = blocks_of(GT)
            def xsrc(o, n):
                return [xs[:, c, g0 + o:g0 + o + n] for c in range(KC)]
            ln_stats(xsrc(*gblocks[0]), [xs], gblocks[0][1], si=0)
            for bi, (o, n) in enumerate(gblocks):
                if bi + 1 < len(gblocks):
                    ln_stats(xsrc(*gblocks[bi + 1]), [xs], gblocks[bi + 1][1], si=(bi + 1) % 2)
                srcs = xsrc(o, n)
                for c in range(KC):
                    ln_apply(srcs[c], xs, n, uT[:, c, o:o + n], uT, op1[:, l * 16 + c:l * 16 + c + 1], md(l, c), [op1, modT], si=bi % 2)
                for c in range(KC):
                    fw.op("act", lambda: A.mul(out=srcs[c], in_=srcs[c], mul=ALPHA), [xs], [xs])

            for c in (range(KC) if "skipB" not in DBG else []):
                s = getw(("s1", l, g, c))
                pp = ypad_v
                gb_t = gbt_v
                if g == 0:
                    fw.op("dve", lambda: V.memset(pp[:, 0:2], 0.0), [], [arena])
                else:
                    fw.op("dve", lambda: V.tensor_copy(out=pp[:, 0:2], in_=histB[:, c, :]), [histB], [arena])
                for (o, n) in gblocks:
                    pgb, pgc, phb = prot.next(), prot.next(), prot.next()
                    rhs = [uT[:, k, o:o + n] for k in range(KC)]
                    mm(pgb[:, 0:n], pgb, [(s[:, k, 0:128], rhs[k]) for k in range(KC)], [s, uT])
                    mm(pgc[:, 0:n], pgc, [(s[:, k, 128:256], rhs[k]) for k in range(KC)], [s, uT])
                    mm(phb[:, 0:n], phb, [(s[:, k, 256:384], rhs[k]) for k in range(KC)], [s, uT])
                    hbv = tmpA.next()
                    fw.op("act", lambda: A.activation(out=hbv[:, 0:n], in_=phb[:, 0:n], func=AF.Identity, bias=col(C_BIN + 32 + c), scale=1.0), [phb, cols], [hbv])
                    fw.op("dve", lambda: V.scalar_tensor_tensor(out=pp[:, 2 + o:2 + o + n], in0=pgc[:, 0:n], scalar=col(C_BIN + 24 + c), in1=hbv[:, 0:n], op0=ALU.add, op1=ALU.mult), [pgc, cols, hbv], [arena])
                    fw.op("act", lambda: A.activation(out=gb_t[:, o:o + n], in_=pgb[:, 0:n], func=AF.Identity, bias=col(C_BIN + 16 + c), scale=1.0), [pgb, cols], [arena])
                if g == 0:
                    fw.op("dve", lambda: V.tensor_tensor(out=pp[:, 2:2 + HALO], in0=pp[:, 2:2 + HALO], in1=maskt[:, :], op=ALU.mult), [arena, maskt], [arena])
                fw.op("dve", lambda: V.tensor_copy(out=histB[:, c, :], in_=pp[:, GT:GT + 2]), [arena], [histB])
                a = acc_v
                cw = C_CBW + c * 3
                fw.op("dve", lambda: V.tensor_scalar(out=a, in0=pp[:, 0:GT], scalar1=col(cw), scalar2=None, op0=ALU.mult), [arena, cols], [arena])
                fw.op("dve", lambda: V.scalar_tensor_tensor(out=a, in0=pp[:, 1:1 + GT], scalar=col(cw + 1), in1=a, op0=ALU.mult, op1=ALU.add), [arena, cols], [arena])
                fw.op("dve", lambda: V.scalar_tensor_tensor(out=a, in0=pp[:, 2:2 + GT], scalar=col(cw + 2), in1=a, op0=ALU.mult, op1=ALU.add), [arena, cols], [arena])
                fw.op("dve", lambda: V.tensor_tensor(out=yab[:, c, :], in0=a, in1=gb_t, op=ALU.mult), [arena], yabB)

            s_pb = getw(("wpb", l, g))
            s_zg = getw(("zgb", l, g), first=("wpb", l, g))
            s_wo = getw(("wo1", l, g), first=("wpb", l, g))
            for bi, (o, n) in enumerate(gblocks if "skipB" not in DBG else []):
                for c in range(KC):
                    py, pz = prot.next(), prot.next()
                    mm(py[:, 0:n], py, [(s_pb[:, k, c * 128:(c + 1) * 128], yab[:, k, o:o + n]) for k in range(KC)], [s_pb, yabB[bi]])
                    mm(pz[:, 0:n], pz, [(s_zg[:, k, c * 128:(c + 1) * 128], uT[:, k, o:o + n]) for k in range(KC)], [s_zg, uT])
                    sg = tmpA.next()
                    fw.op("act", lambda: A.activation(out=sg[:, 0:n], in_=pz[:, 0:n], func=AF.Sigmoid, bias=col(C_BIN + 48 + c), scale=1.0), [pz, cols], [sg])
                    fw.op("dve", lambda: V.tensor_tensor(out=mb_v[:, c, 0:n], in0=py[:, 0:n], in1=sg[:, 0:n], op=ALU.mult), [py, sg], [arena])
                for c in range(KC):
                    po = prot.next()
                    mm(po[:, 0:n], po, [(s_wo[:, k, c * 128:(c + 1) * 128], mb_v[:, k, 0:n]) for k in range(KC)], [s_wo, arena])
                    xa = xs[:, c, g0 + o:g0 + o + n]
                    fw.op("dve", lambda: V.scalar_tensor_tensor(out=xa, in0=po[:, 0:n], scalar=md(l, 16 + c), in1=xa, op0=ALU.mult, op1=ALU.add), [po, modT, xs], [xs])

            for c in (range(KC) if "skipA" not in DBG else []):
                s = getw(("s3", l, g, c))
                yp = ypb_v
                cw = C_CAW + c * 31
                for k in range(31):
                    fw.op("dve", lambda: V.tensor_scalar(out=dg_v[:, k, :], in0=ident_b[:, :], scalar1=col(cw + k), scalar2=None, op0=ALU.mult), [ident_b, cols], [arena])
                if g == 0:
                    fw.op("dve", lambda: V.memset(yp[:, 0:30], 0.0), [], [arena])
                else:
                    fw.op("dve", lambda: V.tensor_copy(out=yp[:, 0:30], in_=histA[:, c, :]), [histA], [arena])
                for (o, n) in gblocks:
                    pv, pg = prot.next(), prot.next()
                    rhs = [uT[:, k, o:o + n] for k in range(KC)]
                    mm(pv[:, 0:n], pv, [(s[:, k, 0:128], rhs[k]) for k in range(KC)], [s, uT])
                    mm(pg[:, 0:n], pg, [(s[:, k, 128:256], rhs[k]) for k in range(KC)], [s, uT])
                    sg = tmpA.next()
                    fw.op("act", lambda: A.activation(out=sg[:, 0:n], in_=pg[:, 0:n], func=AF.Sigmoid, bias=col(C_BIN + 8 + c), scale=1.0), [pg, cols], [sg])
                    fw.op("dve", lambda: V.scalar_tensor_tensor(out=yp[:, 30 + o:30 + o + n], in0=pv[:, 0:n], scalar=col(C_BIN + c), in1=sg[:, 0:n], op0=ALU.add, op1=ALU.mult), [pv, cols, sg], [arena])
                if g == 0:
                    fw.op("dve", lambda: V.tensor_tensor(out=yp[:, 30:30 + HALO], in0=yp[:, 30:30 + HALO], in1=maskt[:, :], op=ALU.mult), [arena, maskt], [arena])
                fw.op("dve", lambda: V.tensor_copy(out=histA[:, c, :], in_=yp[:, GT:GT + 30]), [arena], [histA])
                for bi, (o, n) in enumerate(gblocks):
                    pc = prot.next()
                    mm(pc[:, 0:n], pc, [(dg_v[:, k, :], yp[:, o + k:o + k + n]) for k in range(31)], [arena])
                    fw.op("act", lambda: A.activation(out=yab[:, c, o:o + n], in_=pc[:, 0:n], func=AF.Identity, bias=col(C_CAB + c), scale=1.0), [pc, cols], [yabB[bi]])

            s_pa = getw(("wpa", l, g))
            s_zg = getw(("zga", l, g), first=("wpa", l, g))
            s_wo = getw(("wo2", l, g), first=("wpa", l, g))
            def ysrc(o, n):
                return [yab[:, c, o:o + n] for c in range(KC)]
            if "skipA" not in DBG:
                ln_stats(ysrc(*gblocks[0]), [yabB[0]], gblocks[0][1], bf_sum=True, si=0)
            for bi, (o, n) in enumerate(gblocks if "skipA" not in DBG else []):
                if bi + 1 < len(gblocks):
                    ln_stats(ysrc(*gblocks[bi + 1]), [yabB[bi + 1]], gblocks[bi + 1][1], bf_sum=True, si=(bi + 1) % 2)
                srcs = ysrc(o, n)
                for c in range(KC):
                    ln_apply(srcs[c], yabB[bi], n, sbf_v[:, c, 0:n], arena, col(C_LAG + c), col(C_LAB + c), [cols], func=AF.Silu, si=bi % 2)
                for c in range(KC):
                    py, pz = prot.next(), prot.next()
                    mm(py[:, 0:n], py, [(s_pa[:, k, c * 128:(c + 1) * 128], sbf_v[:, k, 0:n]) for k in range(KC)], [s_pa, arena])
                    mm(pz[:, 0:n], pz, [(s_zg[:, k, c * 128:(c + 1) * 128], uT[:, k, o:o + n]) for k in range(KC)], [s_zg, uT])
                    sg = tmpA.next()
                    fw.op("act", lambda: A.activation(out=sg[:, 0:n], in_=pz[:, 0:n], func=AF.Sigmoid, bias=col(C_BIN + 40 + c), scale=1.0), [pz, cols], [sg])
                    fw.op("dve", lambda: V.scalar_tensor_tensor(out=mb_v[:, c, 0:n], in0=py[:, 0:n], scalar=col(C_BPA + c), in1=sg[:, 0:n], op0=ALU.add, op1=ALU.mult), [py, cols, sg], [arena])
                for c in range(KC):
                    po = prot.next()
                    mm(po[:, 0:n], po, [(s_wo[:, k, c * 128:(c + 1) * 128], mb_v[:, k, 0:n]) for k in range(KC)], [s_wo, arena])
                    xa = xs[:, c, g0 + o:g0 + o + n]
                    fw.op("dve", lambda: V.scalar_tensor_tensor(out=xa, in0=po[:, 0:n], scalar=md(l, 16 + c), in1=xa, op0=ALU.mult, op1=ALU.add), [po, modT, xs], [xs])

            ln_stats(xsrc(*gblocks[0]), [xs], gblocks[0][1], si=0)
            for bi, (o, n) in enumerate(gblocks):
                srcs = xsrc(o, n)
                si = bi % 2
                for c in range(KC):
                    ln_apply(srcs[c], xs, n, srcs[c], xs, col(C_L1G + c), col(C_L1B + c), [cols], si=si)
                if bi + 1 < len(gblocks):
                    ln_stats(xsrc(*gblocks[bi + 1]), [xs], gblocks[bi + 1][1], si=(bi + 1) % 2)
                ln_stats(srcs, [xs], n, si=si)
                for c in range(KC):
                    ln_apply(srcs[c], xs, n, uT[:, c, o:o + n], uT, op1[:, l * 16 + 8 + c:l * 16 + 9 + c], md(l, 24 + c), [op1, modT], si=si)
                for c in range(KC):
                    fw.op("act", lambda: A.mul(out=srcs[c], in_=srcs[c], mul=ALPHA), [xs], [xs])

            fw.dma("sp", b2t_v, b2_d[l], writes=[arena], dname="b2t")
            for o in range(0, GT, 128):
                m = min(128, GT - o)
                pl = pS1
                pairs = [(uT[:, k, o:o + m], rwb[:, k, :]) for k in range(KC)] + [(ones_b[0:1, 0:m], rbb[0:1, :])]
                mm(pl[0:m, 0:NE], pl, pairs, [uT, rwb, ones_b, rbb])
                sm = small.next()
                sm2 = small.next()
                lg, msk, m8, sc = sm[0:m, 0:32], sm[0:m, 32:64], sm[0:m, 64:72], sm[0:m, 72:80]
                ex, em, gt_ = sm2[0:m, 0:32], sm2[0:m, 32:64], sm2[0:m, 64:96]
                fw.op("act", lambda: A.copy(out=lg, in_=pl[0:m, 0:NE]), [pl], [sm])
                fw.op("dve", lambda: V.max(out=m8, in_=lg), [sm], [sm])
                fw.op("dve", lambda: V.tensor_scalar(out=msk, in0=lg, scalar1=m8[:, 3:4], scalar2=None, op0=ALU.is_ge), [sm], [sm])
                fw.op("dve", lambda: V.tensor_scalar(out=sc[:, 0:1], in0=m8[:, 0:1], scalar1=-1.0, scalar2=None, op0=ALU.mult), [sm], [sm])
                fw.op("act", lambda: A.activation(out=ex, in_=lg, func=AF.Exp, bias=sc[:, 0:1], scale=1.0), [sm], [sm2])
                fw.op("dve", lambda: V.tensor_tensor(out=em, in0=ex, in1=msk, op=ALU.mult), [sm, sm2], [sm2])
                fw.op("dve", lambda: V.reduce_sum(out=sc[:, 1:2], in_=em, axis=mybir.AxisListType.X), [sm2], [sm])
                fw.op("dve", lambda: V.reciprocal(out=sc[:, 2:3], in_=sc[:, 1:2]), [sm], [sm])
                fw.op("dve", lambda: V.tensor_scalar(out=gt_, in0=em, scalar1=sc[:, 2:3], scalar2=None, op0=ALU.mult), [sm, sm2], [sm2])
                pt = pS2
                mm(pt[0:NE, 0:m], pt, [(gt_, ident[0:m, 0:m])], [sm2, ident])
                fw.op("act", lambda: A.copy(out=gateT_v[:, o:o + m], in_=pt[0:NE, 0:m]), [pt], [arena])

            for (o, n) in (gblocks if NEXP > 0 else []):
                for c in range(KC):
                    po = prot.next()
                    mm(po[:, 0:n], po, [(b2t_v[:, c * 128:(c + 1) * 128], gateT_v[:, o:o + n])], [arena])
                    xa = xs[:, c, g0 + o:g0 + o + n]
                    fw.op("dve", lambda: V.scalar_tensor_tensor(out=xa, in0=po[:, 0:n], scalar=md(l, 40 + c), in1=xa, op0=ALU.mult, op1=ALU.add), [po, modT, xs], [xs])
            pend = []
            def emit_ge(e):
                for bi, (o, n) in enumerate(gblocks):
                    pge = prot.next()
                    mm(pge[:, 0:n], pge, [(ident[0:NE, e:e + 1].to_broadcast([NE, 128]), gateT_v[:, o:o + n])], [ident, arena])
                    fw.op("act", lambda: A.copy(out=ge_v[bi][:, 0:n], in_=pge[:, 0:n]), [pge], [arena])

            if NEXP > 0:
                emit_ge(0)
            for e in range(NEXP):
                for half, key in ((0, "w1a"), (1, "w1b")):
                    s = getw((key, l, g, e))
                    for bi, (o, n) in enumerate(gblocks):
                        gs = ge_v[bi]
                        for hh in range(4):
                            hc = half * 4 + hh
                            pg_, pl_ = prot.next(), prot.next()
                            rhs = [uT[:, k, o:o + n] for k in range(KC)]
                            mm(pg_[:, 0:n], pg_, [(s[:, k, hh * 128:(hh + 1) * 128], rhs[k]) for k in range(KC)], [s, uT])
                            mm(pl_[:, 0:n], pl_, [(s[:, k, 512 + hh * 128:512 + (hh + 1) * 128], rhs[k]) for k in range(KC)], [s, uT])
                            r2 = tmpD.next()
                            fw.op("act", lambda: A.activation(out=r2[:, 0:n], in_=pg_[:, 0:n], func=AF.Relu, bias=b1s[:, e * 16 + hc:e * 16 + hc + 1], scale=-1.0), [pg_, b1s], [r2])
                            sg = tmpA.next()
                            fw.op("act", lambda: A.activation(out=sg[:, 0:n], in_=r2[:, 0:n], func=AF.Sigmoid, bias=c119[:, 0:1], scale=-1.702), [r2, c119], [sg])
                            lin = tmpL.next()
                            fw.op("dve", lambda: V.tensor_scalar(out=lin[:, 0:n], in0=pl_[:, 0:n], scalar1=b1s[:, e * 16 + 8 + hc:e * 16 + 9 + hc], scalar2=-6.0, op0=ALU.add, op1=ALU.max), [pl_, b1s], [lin])
                            fw.op("dve", lambda: V.scalar_tensor_tensor(out=lin[:, 0:n], in0=lin[:, 0:n], scalar=8.0, in1=gs[:, 0:n], op0=ALU.min, op1=ALU.mult), [lin, arena], [lin])
                            fw.op("act", lambda: A.activation(out=r2[:, 0:n], in_=r2[:, 0:n], func=AF.Identity, bias=c7[:, 0:1], scale=-1.0), [r2, c7], [r2])
                            tp = tmpP.next()
                            fw.op("pool", lambda: G.tensor_tensor(out=tp[:, 0:n], in0=r2[:, 0:n], in1=sg[:, 0:n], op=ALU.mult), [r2, sg], [tp])
                            if pend:
                                pend.pop()()
                            if hc % 2 == 0:
                                fw.op("pool", lambda: G.tensor_tensor(out=yab[:, hc, o:o + n], in0=tp[:, 0:n], in1=lin[:, 0:n], op=ALU.mult), [tp, lin], [yabB[bi]])
                            else:
                                def fin(tp=tp, lin=lin, hc=hc, o=o, n=n, bi=bi):
                                    fw.op("dve", lambda: V.tensor_tensor(out=yab[:, hc, o:o + n], in0=tp[:, 0:n], in1=lin[:, 0:n], op=ALU.mult), [tp, lin], [yabB[bi]])
                                pend.append(fin)
                if pend:
                    pend.pop()()
                s = getw(("w2", l, g, e))
                for bi, (o, n) in enumerate(gblocks):
                    for c in range(KC):
                        po = prot.next()
                        mm(po[:, 0:n], po, [(s[:, k, c * 128:(c + 1) * 128], yab[:, k, o:o + n]) for k in range(KC)], [s, yabB[bi]])
                        xa = xs[:, c, g0 + o:g0 + o + n]
                        fw.op("dve", lambda: V.scalar_tensor_tensor(out=xa, in0=po[:, 0:n], scalar=md(l, 40 + c), in1=xa, op0=ALU.mult, op1=ALU.add), [po, modT, xs], [xs])
                    if bi == 0 and e + 1 < NEXP:
                        emit_ge(e + 1)

            ln_stats(xsrc(*gblocks[0]), [xs], gblocks[0][1], si=0)
            for bi, (o, n) in enumerate(gblocks):
                if bi + 1 < len(gblocks):
                    ln_stats(xsrc(*gblocks[bi + 1]), [xs], gblocks[bi + 1][1], si=(bi + 1) % 2)
                srcs = xsrc(o, n)
                for c in range(KC):
                    ln_apply(srcs[c], xs, n, srcs[c], xs, col(C_L2G + c), col(C_L2B + c), [cols], si=bi % 2)

    d = fw.dma("sp", yT_d, xs[:, :, HALO:T], reads=[xs], dname="out")
    fw._wait("sp", d)
    fw.close()
    return nc


def _pack_cols(inp, L):
    cols = np.zeros((L, 128, NCOL), np.float32)

    def put(l, c0, vec):
        v = np.asarray(vec, np.float32).reshape(-1, 128).T
        cols[l, :, c0:c0 + v.shape[1]] = v

    for l in range(L):
        put(l, C_BIN, inp["b_in"][l])
        caw = np.asarray(inp["conv_a_w"][l], np.float32)
        cols[l, :, C_CAW:C_CAW + 248] = caw.reshape(31, 8, 128).transpose(2, 1, 0).reshape(128, 248)
        put(l, C_CAB, inp["conv_a_b"][l])
        put(l, C_LAG, inp["ln_a_g"][l])
        put(l, C_LAB, inp["ln_a_b"][l])
        cbw = np.asarray(inp["conv_b_w"][l], np.float32)
        cols[l, :, C_CBW:C_CBW + 24] = cbw.reshape(3, 8, 128).transpose(2, 1, 0).reshape(128, 24)
        put(l, C_BPA, inp["b_pa"][l])
        put(l, C_L1G, inp["ln1_g"][l])
        put(l, C_L1B, inp["ln1_b"][l])
        put(l, C_L2G, inp["ln2_g"][l])
        put(l, C_L2B, inp["ln2_b"][l])
        put(l, C_ADB, inp["ada_b"][l])
        put(l, C_B1, np.asarray(inp["b1"][l], np.float32).reshape(-1))
    return cols


_CACHE = {}


def kernel(**inp):
    L = DEPTH
    B, S, _ = inp["x"].shape
    NCORE = 8
    per = (B * S) // NCORE
    cps = S // per
    T = per + HALO
    GT = T // 2
    key = (L, T, GT)
    if key not in _CACHE:
        _CACHE[key] = build_program(L, T, GT)
    nc = _CACHE[key]
    x = np.asarray(inp["x"], np.float32)
    cols = _pack_cols(inp, L)
    ident = np.eye(128, dtype=np.float32)
    shared = dict(
        ident=ident, cols=cols,
        ada_w=np.ascontiguousarray(inp["ada_w"], np.float32), w_in=np.ascontiguousarray(inp["w_in"], np.float32),
        w_pa=np.ascontiguousarray(inp["w_pa"], np.float32), w_pb=np.ascontiguousarray(inp["w_pb"], np.float32),
        w_o=np.ascontiguousarray(inp["w_o"], np.float32), router_w=np.ascontiguousarray(inp["router_w"], np.float32),
        router_b=np.ascontiguousarray(inp["router_b"], np.float32), w1=np.ascontiguousarray(inp["w1"], np.float32),
        w2=np.ascontiguousarray(inp["w2"], np.float32), b2=np.ascontiguousarray(inp["b2"], np.float32),
    )
    in_maps = []
    for core in range(NCORE):
        b, q = divmod(core, cps)
        s0 = q * per
        xt = np.zeros((T, D), np.float32)
        if q == 0:
            xt[HALO:] = x[b, 0:per]
            mask = np.zeros((128, 128), np.float32)
        else:
            xt[:] = x[b, s0 - HALO:s0 + per]
            mask = np.ones((128, 128), np.float32)
        xT = np.ascontiguousarray(xt.T.reshape(KC, 128, T).transpose(1, 0, 2))
        cT = np.ascontiguousarray(np.asarray(inp["c"], np.float32)[b].reshape(KC, 128).T)
        m = dict(shared)
        m.update(xT=xT, cT=cT, mask=mask)
        in_maps.append(m)
    res = run_bass_kernel_spmd(nc, in_maps, core_ids=list(range(NCORE)))
    out = np.zeros((B, S, D), np.float32)
    for core in range(NCORE):
        b, q = divmod(core, cps)
        yT = np.asarray(res.results[core]["yT"])
        out[b, q * per:(q + 1) * per, :] = yT.transpose(2, 1, 0).reshape(per, D)
    return out
```
